# Optimizing a Trainium2 kernel written in Bass

```python
import math
import jax, jax.numpy as jnp
from jax import lax
import numpy as np

D_MODEL = 2048
BATCH = 4
SEQ = 2048
DEPTH = 2

D_MIX = D_MODEL
D_LRU = D_MIX // 2
LRU_HEADS = 4
LRU_HEAD_DIM = D_LRU // LRU_HEADS
CONV_WIDTH = 4
LRU_C = 8.0
D_POOL = D_MIX - D_LRU
POOL_WINDOWS = (2, 4, 8, 16)
N_POOL_GROUPS = len(POOL_WINDOWS)
POOL_GROUP_DIM = D_POOL // N_POOL_GROUPS
D_IN = 2 * D_LRU + D_POOL
D_FF = ((8 * D_MODEL // 3 + 255) // 256) * 256
N_EXPERTS = 8
TOP_K = 2
D_FF_EXPERT = 7 * D_MODEL // 2
N_DENSE = (DEPTH + 1) // 2
N_MOE = DEPTH // 2
DEEPNORM_ALPHA = (2 * DEPTH) ** 0.25
DEEPNORM_BETA = (8 * DEPTH) ** -0.25
LN_EPS = 1e-5

kernel_name = "hybrid_lru_pool_moe_deepnorm_adaln"


def layer_norm(x, g, b):
    xf = x.astype(jnp.float32)
    mu = jnp.mean(xf, axis=-1, keepdims=True)
    var = jnp.mean(jnp.square(xf - mu), axis=-1, keepdims=True)
    return ((xf - mu) * lax.rsqrt(var + LN_EPS) * g.astype(jnp.float32) + b.astype(jnp.float32)).astype(x.dtype)


def adaln_params(c_act, w, b):
    m = c_act @ w + b
    shift, scale, gate = jnp.split(m, 3, axis=-1)
    return shift[:, None, :], scale[:, None, :], gate[:, None, :]


def causal_depthwise_conv(x, w, b):
    S = x.shape[1]
    xp = jnp.pad(x, ((0, 0), (CONV_WIDTH - 1, 0), (0, 0)))
    y = b
    for k in range(CONV_WIDTH):
        y = y + xp[:, k:k + S] * w[k]
    return y


def rg_lru(x, w_a, b_a, w_x, b_x, lam):
    B, S, _ = x.shape
    xh = x.reshape(B, S, LRU_HEADS, LRU_HEAD_DIM)
    gate_a = jax.nn.sigmoid(jnp.einsum('bshi,hij->bshj', xh, w_a).reshape(B, S, D_LRU) + b_a)
    gate_x = jax.nn.sigmoid(jnp.einsum('bshi,hij->bshj', xh, w_x).reshape(B, S, D_LRU) + b_x)
    log_a = -LRU_C * gate_a.astype(jnp.float32) * jax.nn.softplus(-lam.astype(jnp.float32))
    a = jnp.exp(log_a)
    mult = jnp.sqrt(-jnp.expm1(2.0 * log_a))
    u = (x * gate_x).astype(jnp.float32) * mult

    def step(h, au):
        a_t, u_t = au
        h = a_t * h + u_t
        return h, h

    h0 = jnp.zeros((B, D_LRU), jnp.float32)
    _, hs = lax.scan(step, h0, (jnp.swapaxes(a, 0, 1), jnp.swapaxes(u, 0, 1)))
    return jnp.swapaxes(hs, 0, 1).astype(x.dtype)


def multiscale_pool(x, w_pool, b_pool, scale):
    B, S, _ = x.shape
    xg = x.astype(jnp.float32).reshape(B, S, N_POOL_GROUPS, POOL_GROUP_DIM)
    cs = jnp.cumsum(xg, axis=1)
    outs = []
    for gi, w in enumerate(POOL_WINDOWS):
        c_g = cs[:, :, gi]
        lag = jnp.pad(c_g, ((0, 0), (w, 0), (0, 0)))[:, :S]
        cnt = jnp.minimum(jnp.arange(1, S + 1), w).astype(jnp.float32)[None, :, None]
        outs.append((c_g - lag) / cnt - xg[:, :, gi])
    p = jnp.stack(outs, axis=2).astype(x.dtype)
    y = jnp.einsum('bsgi,gij->bsgj', p, w_pool) + b_pool
    return y.reshape(B, S, D_POOL) * scale


def hybrid_mixer(h, w_in, conv_w, conv_b, lru_wa, lru_ba, lru_wx, lru_bx, lru_lam,
                 pool_w, pool_b, pool_scale, w_out):
    z = h @ w_in
    x_lru, g_lru, x_pool = jnp.split(z, [D_LRU, 2 * D_LRU], axis=-1)
    x_lru = causal_depthwise_conv(x_lru, conv_w, conv_b)
    y_lru = rg_lru(x_lru, lru_wa, lru_ba, lru_wx, lru_bx, lru_lam) * jax.nn.gelu(g_lru)
    y_pool = multiscale_pool(x_pool, pool_w, pool_b, pool_scale)
    return jnp.concatenate([y_lru, y_pool], axis=-1) @ w_out


def swiglu(h, w_gate, w_up, w_down):
    return (jax.nn.silu(h @ w_gate) * (h @ w_up)) @ w_down


def moe_swiglu(h, w_router, w_gate, w_up, w_down):
    B, S, D = h.shape
    t = h.reshape(B * S, D)
    logits = (t @ w_router).astype(jnp.float32)
    top_v, top_i = lax.top_k(logits, TOP_K)
    probs = jax.nn.softmax(top_v, axis=-1)
    combine = jnp.einsum('nk,nke->ne', probs, jax.nn.one_hot(top_i, N_EXPERTS, dtype=jnp.float32))
    out = jnp.zeros_like(t)
    for e in range(N_EXPERTS):
        y = swiglu(t, w_gate[e], w_up[e], w_down[e])
        out = out + combine[:, e:e + 1].astype(t.dtype) * y
    return out.reshape(B, S, D)


def setup_inputs(seed: int = 0) -> dict:
    key = jax.random.key(seed)
    ks = jax.random.split(key, 26)
    n = lambda k, shape, s: jax.random.normal(k, shape, jnp.float32) * s
    u = jax.random.uniform(ks[13], (DEPTH, D_LRU), jnp.float32, 0.81, 0.998)
    p = u ** (1.0 / LRU_C)
    lru_lam = jnp.log(p) - jnp.log1p(-p)
    return {
        "x": n(ks[0], (BATCH, SEQ, D_MODEL), 1.0),
        "c": n(ks[1], (BATCH, D_MODEL), 1.0),
        "ada_w": n(ks[2], (DEPTH, 2, D_MODEL, 3 * D_MODEL), 0.1 * D_MODEL ** -0.5),
        "ada_b": n(ks[3], (DEPTH, 2, 3 * D_MODEL), 0.01),
        "ln_g": 1.0 + n(ks[4], (DEPTH, 2, D_MODEL), 0.02),
        "ln_b": n(ks[5], (DEPTH, 2, D_MODEL), 0.02),
        "mix_w_in": n(ks[6], (DEPTH, D_MODEL, D_IN), D_MODEL ** -0.5),
        "conv_w": n(ks[7], (DEPTH, CONV_WIDTH, D_LRU), CONV_WIDTH ** -0.5),
        "conv_b": n(ks[8], (DEPTH, D_LRU), 0.01),
        "lru_wa": n(ks[9], (DEPTH, LRU_HEADS, LRU_HEAD_DIM, LRU_HEAD_DIM), LRU_HEAD_DIM ** -0.5),
        "lru_ba": n(ks[10], (DEPTH, D_LRU), 0.01),
        "lru_wx": n(ks[11], (DEPTH, LRU_HEADS, LRU_HEAD_DIM, LRU_HEAD_DIM), LRU_HEAD_DIM ** -0.5),
        "lru_bx": n(ks[12], (DEPTH, D_LRU), 0.01),
        "lru_lam": lru_lam,
        "pool_w": n(ks[14], (DEPTH, N_POOL_GROUPS, POOL_GROUP_DIM, POOL_GROUP_DIM), POOL_GROUP_DIM ** -0.5),
        "pool_b": n(ks[15], (DEPTH, N_POOL_GROUPS, POOL_GROUP_DIM), 0.01),
        "pool_scale": 1.0 + n(ks[16], (DEPTH, D_POOL), 0.02),
        "mix_w_out": n(ks[17], (DEPTH, D_MIX, D_MODEL), DEEPNORM_BETA * D_MIX ** -0.5),
        "ffn_w_gate": n(ks[18], (N_DENSE, D_MODEL, D_FF), D_MODEL ** -0.5),
        "ffn_w_up": n(ks[19], (N_DENSE, D_MODEL, D_FF), D_MODEL ** -0.5),
        "ffn_w_down": n(ks[20], (N_DENSE, D_FF, D_MODEL), DEEPNORM_BETA * D_FF ** -0.5),
        "router_w": n(ks[21], (N_MOE, D_MODEL, N_EXPERTS), D_MODEL ** -0.5),
        "exp_w_gate": n(ks[22], (N_MOE, N_EXPERTS, D_MODEL, D_FF_EXPERT), D_MODEL ** -0.5),
        "exp_w_up": n(ks[23], (N_MOE, N_EXPERTS, D_MODEL, D_FF_EXPERT), D_MODEL ** -0.5),
        "exp_w_down": n(ks[24], (N_MOE, N_EXPERTS, D_FF_EXPERT, D_MODEL), DEEPNORM_BETA * D_FF_EXPERT ** -0.5),
    }


def reference(x, c, ada_w, ada_b, ln_g, ln_b, mix_w_in, conv_w, conv_b, lru_wa, lru_ba,
              lru_wx, lru_bx, lru_lam, pool_w, pool_b, pool_scale, mix_w_out,
              ffn_w_gate, ffn_w_up, ffn_w_down, router_w, exp_w_gate, exp_w_up, exp_w_down):
    c_act = jax.nn.silu(c)
    for l in range(DEPTH):
        shift, scale, gate = adaln_params(c_act, ada_w[l, 0], ada_b[l, 0])
        h = x * (1.0 + scale) + shift
        o = hybrid_mixer(h, mix_w_in[l], conv_w[l], conv_b[l], lru_wa[l], lru_ba[l],
                         lru_wx[l], lru_bx[l], lru_lam[l], pool_w[l], pool_b[l],
                         pool_scale[l], mix_w_out[l])
        x = layer_norm(DEEPNORM_ALPHA * x + (1.0 + gate) * o, ln_g[l, 0], ln_b[l, 0])
        shift, scale, gate = adaln_params(c_act, ada_w[l, 1], ada_b[l, 1])
        h = x * (1.0 + scale) + shift
        if l % 2 == 0:
            i = l // 2
            o = swiglu(h, ffn_w_gate[i], ffn_w_up[i], ffn_w_down[i])
        else:
            i = l // 2
            o = moe_swiglu(h, router_w[i], exp_w_gate[i], exp_w_up[i], exp_w_down[i])
        x = layer_norm(DEEPNORM_ALPHA * x + (1.0 + gate) * o, ln_g[l, 1], ln_b[l, 1])
    return x
```

```python
import numpy as np
import concourse.bass as bass
import concourse.mybir as mybir
from concourse.bass_utils import run_bass_kernel_spmd
from contextlib import ExitStack

F32 = mybir.dt.float32
BF16 = mybir.dt.bfloat16
AF = mybir.ActivationFunctionType
ALU = mybir.AluOpType

D = 2048
NCD = 16
T = 1024
TT = 512
NTT = 2
HALO = 16
D_LRU = 1024
D_FF = 5632
D_FFE = 7168
NEXP = 8
ALPHA = (2 * 2) ** 0.25
LN_EPS = 1e-5
EPS2 = LN_EPS / (ALPHA * ALPHA)
WINS = (2, 4, 8, 16)
NS = 4
GD = 4
GE = 8

N_CORES = 4
DBG = {}


class Buf:
    __slots__ = ("w", "r")

    def __init__(self):
        self.w = None
        self.r = {}


class Eng:
    def __init__(self, name, sem):
        self.name = name
        self.sem = sem
        self.cnt = 0
        self.ops = []
        self.waited = {}
        self.pending = []


class Prog:
    def __init__(self, nc, es):
        self.nc = nc
        self.es = es
        self.engs = {}
        for n in ("pe", "act", "dve", "pool", "sp"):
            sem = es.enter_context(nc.semaphore("s_" + n))
            self.engs[n] = Eng(n, sem)
        self.semid = {}

    def _deps(self, eng, reads, writes, extra):
        deps = {}

        def add(t):
            if t is None:
                return
            k = id(t[0])
            if k not in deps or deps[k][1] < t[1]:
                deps[k] = t

        for b in reads:
            add(b.w)
        for b in writes:
            add(b.w)
            for t in b.r.values():
                add(t)
        for t in extra:
            add(t)
        for t in eng.pending:
            add(t)
        eng.pending = []
        waits = []
        for k, t in deps.items():
            sem, val, src = t
            if src is eng and eng.name == "pe":
                continue
            if eng.waited.get(k, 0) >= val:
                continue
            eng.waited[k] = val
            waits.append((sem, val))
        return waits

    def _commit(self, ticket, reads, writes):
        for b in reads:
            b.r[id(ticket[0])] = ticket
        for b in writes:
            b.w = ticket
            b.r = {}

    def op(self, en, fn, reads=(), writes=(), signal=True, extra=()):
        eng = self.engs[en]
        waits = self._deps(eng, reads, writes, extra)
        if signal:
            eng.cnt += 1
            ticket = (eng.sem, eng.cnt, eng)
        else:
            ticket = (eng.sem, eng.cnt + 1, eng)
        eng.ops.append((waits, fn, (eng.sem, 1) if signal else None))
        self._commit(ticket, reads, writes)
        return ticket

    def dma(self, en, dsem, out, in_, reads=(), writes=(), extra=()):
        eng = self.engs[en]
        waits = self._deps(eng, reads, writes, extra)
        dsem[1] += 16
        ticket = (dsem[0], dsem[1], None)
        eng.ops.append((waits, lambda e: e.dma_start(out=out, in_=in_), (dsem[0], 16)))
        self._commit(ticket, reads, writes)
        return ticket

    def sync_all(self):
        ts = []
        for e in self.engs.values():
            if e.cnt > 0:
                ts.append((e.sem, e.cnt, e))
        for e in self.engs.values():
            e.pending = [t for t in ts if t[2] is not e]

    def emit(self, block):
        nc = self.nc
        amap = {"pe": "tensor", "act": "scalar", "dve": "vector", "pool": "gpsimd", "sp": "sync"}
        for n, eng in self.engs.items():
            ops = eng.ops

            def body(e, ops=ops):
                for waits, fn, sig in ops:
                    for sem, val in waits:
                        e.wait_ge(sem, val)
                    inst = fn(e)
                    if sig is not None:
                        inst.then_inc(sig[0], sig[1])

            getattr(block, amap[n])(body)


def pack_cols(W, order=None):
    K, M = W.shape
    A = W.reshape(K // 128, 128, M // 128, 128).transpose(2, 1, 0, 3).reshape(M // 128, 128, (K // 128) * 128)
    if order is not None:
        A = A[np.asarray(order)]
    return np.ascontiguousarray(A)


def pack_down(W, G):
    F, M = W.shape
    A = W.reshape(F // (128 * G), G, 128, M // 128, 128).transpose(0, 3, 2, 1, 4)
    return np.ascontiguousarray(A.reshape(-1, 128, G * 128))


def pack_sq(W):
    return np.ascontiguousarray(W.reshape(2, 128, 256).transpose(1, 0, 2).reshape(128, 512))


def fm(v):
    return np.ascontiguousarray(v.reshape(-1, 128).T)


WIN_ORDER = []
for _hd in range(4):
    WIN_ORDER += [2 * _hd, 2 * _hd + 1, 8 + 2 * _hd, 8 + 2 * _hd + 1]
for _g in range(4):
    WIN_ORDER += [16 + 2 * _g, 16 + 2 * _g + 1]


class PV:
    def __init__(self):
        self.cols = []
        self.off = {}
        self.n = 0

    def add(self, name, arr):
        arr = np.asarray(arr, np.float32)
        assert arr.shape[0] == 128
        self.off[name] = (self.n, arr.shape[1])
        self.cols.append(arr)
        self.n += arr.shape[1]


def build_pvec(inp, seqs, layers):
    pv = PV()
    for i, b in enumerate(seqs):
        pv.add(f"c{i}", fm(inp["c"][b]))
    for l in layers:
        for s in range(2):
            pv.add(f"adab{l}{s}", fm(inp["ada_b"][l, s]))
            pv.add(f"lng{l}{s}", fm(inp["ln_g"][l, s]))
            pv.add(f"lnb{l}{s}", fm(inp["ln_b"][l, s]))
        cw = inp["conv_w"][l]
        pv.add(f"convw{l}", np.concatenate([fm(cw[k])[:, :, None] for k in range(4)], axis=2).reshape(128, 32))
        pv.add(f"convb{l}", fm(inp["conv_b"][l]))
        pv.add(f"ba{l}", fm(inp["lru_ba"][l]))
        pv.add(f"bx{l}", fm(inp["lru_bx"][l]))
        pv.add(f"lam{l}", fm(inp["lru_lam"][l]))
        pv.add(f"poolb{l}", fm(inp["pool_b"][l].reshape(-1)))
        pv.add(f"pools{l}", fm(inp["pool_scale"][l]))
    ic = np.zeros((4, 16), np.float32)
    for g, w in enumerate(WINS):
        ic[g] = 1.0 / np.minimum(np.arange(1, 17), w)
    pv.add("invcnt", np.broadcast_to(ic.reshape(1, 64), (128, 64)))
    rw = inp["router_w"][0]
    pv.add("router", rw.reshape(16, 128, 8).transpose(1, 0, 2).reshape(128, 128))
    pv.add("ident", np.eye(128, dtype=np.float32))
    pv.add("ones", np.ones((128, 128), np.float32))
    return pv


def build_program(n_tiles_seq, n_seq, layers, wshapes, npv, pvoff):
    nc = bass.Bass("TRN2", target_bir_lowering=False)
    n_tiles = n_seq * n_tiles_seq
    xin = nc.dram_tensor("xin", [n_tiles, 128, NCD * T], F32, kind="ExternalInput").ap()
    yout = nc.dram_tensor("yout", [n_tiles, 128, NCD * T], F32, kind="ExternalOutput").ap()
    pvd = nc.dram_tensor("pvec", [128, npv], F32, kind="ExternalInput").ap()
    ymd = nc.dram_tensor("ymd", [128, NCD * T], BF16, kind="ExternalOutput").ap() if DBG.get("dump_ym") else None
    wd = {}
    for name, shp in wshapes.items():
        wd[name] = nc.dram_tensor(name, list(shp), F32, kind="ExternalInput").ap()

    es = ExitStack()
    P = Prog(nc, es)

    def sb(name, shape, dt):
        return es.enter_context(nc.sbuf_tensor(name, list(shape), dt))

    xb = sb("xb", [128, NCD, T], F32)
    hb = sb("hb", [128, NCD, T], BF16)
    ym = sb("ym", [128, NCD * T], BF16)
    wr = sb("wr", [128, NS, 2048], BF16)
    NSCR = 8192
    scr = sb("scr", [128, NSCR], F32)
    gg = sb("gg", [128, 2, T], BF16)
    xcb = sb("xcb", [128, 2, T], BF16)
    ftm = sb("ftm", [128, 4, TT], F32)
    msc = sb("msc", [128, 320], F32)
    pvt = sb("pvt", [128, npv], F32)
    modp = sb("modp", [128, 4, 48], F32)
    der = sb("der", [128, 4, 32], F32)
    lrc = sb("lrc", [128, 2, 3, 8], F32)
    cact = sb("cact", [128, 16], BF16)
    czx = sb("czx", [128, 2, 8, HALO], F32)
    czp = sb("czp", [128, 2, 8, HALO], F32)
    chh = sb("chh", [128, 2, 8], F32)
    rtm = sb("rtm", [128, 8, 8], F32)
    banks = [es.enter_context(nc.psum_tensor(f"bk{i}", [128, 512], F32)) for i in range(8)]

    def carve(off, shape):
        n = int(np.prod(shape))
        v = scr[:, off:off + n]
        if len(shape) == 2:
            v = v.rearrange("p (a b) -> p a b", a=shape[0], b=shape[1])
        return v, off + n

    o = 0
    zx, o = carve(o, [2, T + HALO])
    xc, o0 = carve(o, [2, T])
    mt = []
    o1 = o0
    for i in range(6):
        v, o1 = carve(o1, [TT])
        mt.append(v)
    assert o1 <= NSCR, o1
    sA, o2 = carve(o, [T + HALO])
    sB, o2 = carve(o2, [T + HALO])
    assert o2 <= o1
    pb = gg
    tfix = msc[:, 288:304]
    cwb, _ = carve(0, [NEXP, T])
    o3 = 0
    lnt = []
    for i in range(7):
        v, o3 = carve(o3, [TT])
        lnt.append(v)

    B = lambda: Buf()
    bx = [[B() for _ in range(NTT)] for _ in range(NCD)]
    bh = [B() for _ in range(NCD)]
    bym = [B() for _ in range(NCD)]
    bh1 = [[B() for _ in range(GE)] for _ in range(2)]
    bslot = [B() for _ in range(NS)]
    bbank = [B() for _ in range(8)]
    bpv = B()
    bmod = B()
    bder = B()
    blrc = B()
    bcact = B()
    bczx = [[B() for _ in range(8)] for _ in range(2)]
    bczp = [[B() for _ in range(8)] for _ in range(2)]
    bchh = [[B() for _ in range(8)] for _ in range(2)]
    bscr = {}

    def SB(name):
        if name not in bscr:
            bscr[name] = B()
        return bscr[name]

    def pvc(name, c0=0, n=None):
        off, w = pvoff[name]
        if n is None:
            n = w - c0
        return pvt[:, off + c0: off + c0 + n]

    plan = []

    def plan_build():
        for sq in range(n_seq):
            for tl in range(n_tiles_seq):
                if tl == 0:
                    for l in layers:
                        for s in range(2):
                            for m in range(48):
                                plan.append(("ada", (l * 2 + s) * 48 + m, 2048))
                for l in layers:
                    for hd in range(4):
                        for j in range(4):
                            plan.append(("win", l * 24 + hd * 4 + j, 2048))
                        plan.append(("gat", l * 12 + hd * 2, 512))
                        plan.append(("gat", l * 12 + hd * 2 + 1, 512))
                    for g in range(4):
                        for j in range(2):
                            plan.append(("win", l * 24 + 16 + g * 2 + j, 2048))
                        plan.append(("gat", l * 12 + 8 + g, 512))
                    for m in range(16):
                        plan.append(("wout", l * 16 + m, 2048))
                    if DBG.get('skip_ffn'):
                        pass
                    elif l == 0:
                        for grp in range(D_FF // 128 // GD):
                            for kk in range(GD):
                                fc = grp * GD + kk
                                plan.append(("fgu", fc * 2, 2048))
                                plan.append(("fgu", fc * 2 + 1, 2048))
                            for m in range(16):
                                plan.append(("fdn", grp * 16 + m, GD * 128))
                    else:
                        for e in range(NEXP):
                            for grp in range(D_FFE // 128 // GE):
                                for kk in range(GE):
                                    fc = grp * GE + kk
                                    plan.append(("egu", (e * 56 + fc) * 2, 2048))
                                    plan.append(("egu", (e * 56 + fc) * 2 + 1, 2048))
                                for m in range(16):
                                    plan.append(("edn", (e * 7 + grp) * 16 + m, GE * 128))

    plan_build()
    wst = {"issued": 0, "next": 0}
    wsems = [[es.enter_context(nc.semaphore(f"ws{i}")), 0] for i in range(NS)]

    def wget(kind, idx, cols):
        i = wst["next"]
        assert plan[i] == (kind, idx, cols), (i, plan[i], (kind, idx, cols))
        while wst["issued"] < min(len(plan), i + NS - 1):
            j = wst["issued"]
            k2, i2, c2 = plan[j]
            s = j % NS
            P.dma("pool", wsems[s], wr[:, s, 0:c2], wd[k2][i2], reads=(), writes=(bslot[s],))
            wst["issued"] += 1
        wst["next"] += 1
        s = i % NS
        return wr[:, s, :], bslot[s]

    bank_i = [0]

    def getbank():
        i = bank_i[0] % 8
        bank_i[0] += 1
        return banks[i], bbank[i]

    def mm(out, lhsT, rhs, start, stop, reads, writes, signal):
        P.op("pe", lambda e: e.matmul(out, lhsT, rhs, start=start, stop=stop),
             reads=reads, writes=writes, signal=signal)

    def act(out, in_, func, reads, writes, bias=None, scale=None):
        kw = {}
        if bias is not None:
            kw["bias"] = bias
        if scale is not None:
            kw["scale"] = scale
        P.op("act", lambda e: e.activation(out=out, in_=in_, func=func, **kw), reads=reads, writes=writes)

    def tt_(out, in0, in1, op, reads, writes):
        P.op("dve", lambda e: e.tensor_tensor(out=out, in0=in0, in1=in1, op=op), reads=reads, writes=writes)

    def stt(out, in0, scalar, in1, op0, op1, reads, writes):
        P.op("dve", lambda e: e.scalar_tensor_tensor(out=out, in0=in0, scalar=scalar, in1=in1, op0=op0, op1=op1),
             reads=reads, writes=writes)

    def ts(out, in0, s1, s2, op0, op1, reads, writes):
        if op1 is None:
            P.op("dve", lambda e: e.tensor_scalar(out=out, in0=in0, scalar1=s1, scalar2=None, op0=op0),
                 reads=reads, writes=writes)
        else:
            P.op("dve", lambda e: e.tensor_scalar(out=out, in0=in0, scalar1=s1, scalar2=s2, op0=op0, op1=op1),
                 reads=reads, writes=writes)

    dsm = [es.enter_context(nc.semaphore("dsm")), 0]
    P.dma("sp", dsm, pvt[:, :], pvd[:, :], writes=(bpv,))
    for li, l in enumerate(layers):
        yv, zv, z2, acc = rtm[:, 0, :], rtm[:, 1, :], rtm[:, 2, :], rtm[:, 3, :]
        act(yv, pvc(f"lam{l}"), AF.Exp, (bpv,), (SB("rtm0"),), scale=-1.0)
        ts(zv, yv, 2.0, None, ALU.add, None, (SB("rtm0"),), (SB("rtm1"),))
        P.op("dve", lambda e, zv=zv: e.reciprocal(out=zv, in_=zv), reads=(SB("rtm1"),), writes=(SB("rtm1"),))
        tt_(zv, zv, yv, ALU.mult, (SB("rtm1"), SB("rtm0")), (SB("rtm1"),))
        tt_(z2, zv, zv, ALU.mult, (SB("rtm1"),), (SB("rtm2"),))
        ts(acc, z2, 1.0 / 13.0, 1.0 / 11.0, ALU.mult, ALU.add, (SB("rtm2"),), (SB("rtm3"),))
        for cf in (1.0 / 9.0, 1.0 / 7.0, 1.0 / 5.0, 1.0 / 3.0, 1.0):
            tt_(acc, acc, z2, ALU.mult, (SB("rtm3"), SB("rtm2")), (SB("rtm3"),))
            ts(acc, acc, cf, None, ALU.add, None, (SB("rtm3"),), (SB("rtm3"),))
        tt_(acc, acc, zv, ALU.mult, (SB("rtm3"), SB("rtm1")), (SB("rtm3"),))
        tmp = acc
        ts(lrc[:, l, 0, :], tmp, -16.0, None, ALU.mult, None, (SB("rtm3"),), (blrc,))
        ts(lrc[:, l, 1, :], tmp, -2.0, None, ALU.mult, None, (SB("rtm3"),), (blrc,))
        tt_(lrc[:, l, 2, :], pvc(f"poolb{l}"), pvc(f"pools{l}"), ALU.mult, (bpv,), (blrc,))

    def modulate(sidx, dst_is_bf=True):
        for c in range(NCD):
            act(hb[:, c, :], xb[:, c, :], AF.Identity, (bx[c][0], bx[c][1], bder, bmod), (bh[c],),
                bias=modp[:, sidx, c:c + 1], scale=der[:, sidx, c:c + 1])

    def layer_norm(l, s):
        sidx_g = pvc(f"lng{l}{s}")
        sidx_b = pvc(f"lnb{l}{s}")
        ones = pvc("ones")
        sq = [lnt[0], lnt[1]]
        mean_t, msq_t, rstd_t = lnt[2], lnt[3], lnt[4]
        tmpv = [lnt[5], lnt[6]]
        for t in range(NTT):
            tsl = slice(t * TT, (t + 1) * TT)
            b1, bb1 = getbank()
            b2, bb2 = getbank()
            for c in range(NCD):
                mm(b1[:, :], ones, xb[:, c, tsl], c == 0, c == NCD - 1, (bx[c][t], bpv), (bb1,), c == NCD - 1)
                act(sq[c % 2], xb[:, c, tsl], AF.Square, (bx[c][t],), (SB(f"lnsq{c % 2}"),))
                mm(b2[:, :], ones, sq[c % 2], c == 0, c == NCD - 1, (SB(f"lnsq{c % 2}"), bpv), (bb2,), True)
            act(mean_t, b1[:, :], AF.Identity, (bb1,), (SB("lnmean"),), scale=1.0 / D)
            tt_(msq_t, mean_t, mean_t, ALU.mult, (SB("lnmean"),), (SB("lnmsq"),))
            stt(msq_t, b2[:, :], 1.0 / D, msq_t, ALU.mult, ALU.subtract, (bb2, SB("lnmsq")), (SB("lnmsq"),))
            act(rstd_t, msq_t, AF.Sqrt, (SB("lnmsq"),), (SB("lnrstd"),), bias=EPS2)
            P.op("dve", lambda e: e.reciprocal(out=rstd_t, in_=rstd_t), reads=(SB("lnrstd"),), writes=(SB("lnrstd"),))
            for c in range(NCD):
                tv = tmpv[c % 2]
                tb = SB(f"lntmp{c % 2}")
                tt_(tv, xb[:, c, tsl], mean_t, ALU.subtract, (bx[c][t], SB("lnmean")), (tb,))
                tt_(tv, tv, rstd_t, ALU.mult, (tb, SB("lnrstd")), (tb,))
                act(xb[:, c, tsl], tv, AF.Identity, (tb, bpv), (bx[c][t],),
                    bias=sidx_b[:, c:c + 1], scale=sidx_g[:, c:c + 1])

    def adaln(seq_i):
        act(cact[:, :], pvc(f"c{seq_i}"), AF.Silu, (bpv,), (bcact,))
        for li, l in enumerate(layers):
            for s in range(2):
                sidx = l * 2 + s
                bk, bbk = getbank()
                for m in range(48):
                    wt, wb = wget("ada", sidx * 48 + m, 2048)
                    for k in range(NCD):
                        mm(bk[:, m:m + 1], wt[:, k * 128:(k + 1) * 128], cact[:, k:k + 1], k == 0, k == NCD - 1,
                           (wb, bcact), (bbk,), (k == NCD - 1))
                tt_(modp[:, sidx, :], bk[:, 0:48], pvc(f"adab{l}{s}"), ALU.add, (bbk, bpv), (bmod,))
                ts(der[:, sidx, 0:16], modp[:, sidx, 16:32], 1.0, None, ALU.add, None, (bmod,), (bder,))
                ts(der[:, sidx, 16:32], modp[:, sidx, 32:48], 1.0, 1.0 / ALPHA, ALU.add, ALU.mult, (bmod,), (bder,))

    def residual_from_bank(bk, bbk, sidx, m, t):
        tsl = slice(t * TT, (t + 1) * TT)
        stt(xb[:, m, tsl], bk[:, :], der[:, sidx, 16 + m:17 + m], xb[:, m, tsl], ALU.mult, ALU.add,
            (bbk, bder, bx[m][t]), (bx[m][t],))

    def mixer(l, first):
        sidx = l * 2
        modulate(sidx)
        convw = pvc(f"convw{l}")
        for hd in range(4):
            for ci in range(2):
                ch = 2 * hd + ci
                if first:
                    P.op("dve", lambda e, ci=ci: e.memset(zx[:, ci, 0:HALO], 0.0), writes=(SB(f"zx{ci}"),))
                else:
                    act(zx[:, ci, 0:HALO], czx[:, l, ch, :], AF.Identity, (bczx[l][ch],), (SB(f"zx{ci}"),))
                wt, wb = wget("win", l * 24 + hd * 4 + ci, 2048)
                for t in range(NTT):
                    bk, bbk = getbank()
                    for k in range(NCD):
                        mm(bk[:, :], wt[:, k * 128:(k + 1) * 128], hb[:, k, t * TT:(t + 1) * TT], k == 0, k == NCD - 1,
                           (wb, bh[k]), (bbk,), k == NCD - 1)
                    act(zx[:, ci, HALO + t * TT: HALO + (t + 1) * TT], bk[:, :], AF.Identity, (bbk,), (SB(f"zx{ci}"),))
                act(czx[:, l, ch, :], zx[:, ci, T:T + HALO], AF.Identity, (SB(f"zx{ci}"),), (bczx[l][ch],))
            for ci in range(2):
                wt, wb = wget("win", l * 24 + hd * 4 + 2 + ci, 2048)
                for t in range(NTT):
                    bk, bbk = getbank()
                    for k in range(NCD):
                        mm(bk[:, :], wt[:, k * 128:(k + 1) * 128], hb[:, k, t * TT:(t + 1) * TT], k == 0, k == NCD - 1,
                           (wb, bh[k]), (bbk,), k == NCD - 1)
                    act(gg[:, ci, t * TT:(t + 1) * TT], bk[:, :], AF.Gelu_apprx_tanh, (bbk,), (SB(f"gg{ci}"),))
            for ci in range(2):
                ch = 2 * hd + ci
                act(xc[:, ci, :], zx[:, ci, HALO:HALO + T], AF.Identity, (SB(f"zx{ci}"), bpv), (SB(f"xc{ci}"),),
                    bias=pvc(f"convb{l}", ch, 1), scale=convw[:, ch * 4 + 3: ch * 4 + 4])
                for dd in (1, 2, 3):
                    stt(xc[:, ci, :], zx[:, ci, HALO - dd:HALO - dd + T], convw[:, ch * 4 + 3 - dd: ch * 4 + 4 - dd],
                        xc[:, ci, :], ALU.mult, ALU.add, (SB(f"zx{ci}"), bpv, SB(f"xc{ci}")), (SB(f"xc{ci}"),))
                act(xcb[:, ci, :], xc[:, ci, :], AF.Identity, (SB(f"xc{ci}"),), (SB(f"xcb{ci}"),))
            wa, wab = wget("gat", l * 12 + hd * 2, 512)
            wx, wxb = wget("gat", l * 12 + hd * 2 + 1, 512)
            for co in range(2):
                ch = 2 * hd + co
                for t in range(NTT):
                    tsl = slice(t * TT, (t + 1) * TT)
                    ba_, bba = getbank()
                    bx_, bbx = getbank()
                    for ci in range(2):
                        mm(ba_[:, :], wa[:, ci * 256 + co * 128: ci * 256 + co * 128 + 128], xcb[:, ci, tsl],
                           ci == 0, ci == 1, (wab, SB(f"xcb{ci}")), (bba,), ci == 1)
                    for ci in range(2):
                        mm(bx_[:, :], wx[:, ci * 256 + co * 128: ci * 256 + co * 128 + 128], xcb[:, ci, tsl],
                           ci == 0, ci == 1, (wxb, SB(f"xcb{ci}")), (bbx,), ci == 1)
                    r_, gx_, a2_, a_, u_, hs_ = mt
                    act(r_, ba_[:, :], AF.Sigmoid, (bba, bpv), (SB("r"),), bias=pvc(f"ba{l}", ch, 1))
                    act(gx_, bx_[:, :], AF.Sigmoid, (bbx, bpv), (SB("gx"),), bias=pvc(f"bx{l}", ch, 1))
                    if DBG.get('dump_r') == 'r':
                        act(ym[:, ch * T + t * TT: ch * T + (t + 1) * TT], r_, AF.Identity, (SB("r"),), (bym[ch],))
                    if DBG.get('dump_r') == 'gx':
                        act(ym[:, ch * T + t * TT: ch * T + (t + 1) * TT], gx_, AF.Identity, (SB("gx"),), (bym[ch],))
                    if DBG.get('dump_r') == 'xc':
                        act(ym[:, ch * T + t * TT: ch * T + (t + 1) * TT], xc[:, co, tsl], AF.Identity, (SB(f"xc{co}"),), (bym[ch],))
                    ts(r_, r_, lrc[:, l, 1, ch:ch + 1], None, ALU.mult, None, (SB("r"), blrc), (SB("r"),))
                    ts(a2_, r_, 1.0 / 720.0, None, ALU.mult, None, (SB("r"),), (SB("a2"),))
                    for ck in (1.0 / 120.0, 1.0 / 24.0, 1.0 / 6.0, 0.5, 1.0):
                        stt(a2_, a2_, ck, r_, ALU.add, ALU.mult, (SB("a2"), SB("r")), (SB("a2"),))
                    for _ in range(3):
                        stt(a2_, a2_, 2.0, a2_, ALU.add, ALU.mult, (SB("a2"),), (SB("a2"),))
                    ts(a_, a2_, 1.0, None, ALU.add, None, (SB("a2"),), (SB("a"),))
                    stt(a2_, a2_, 2.0, a2_, ALU.add, ALU.mult, (SB("a2"),), (SB("a2"),))
                    act(a2_, a2_, AF.Sqrt, (SB("a2"),), (SB("a2"),), scale=-1.0)
                    tt_(u_, xc[:, co, tsl], gx_, ALU.mult, (SB(f"xc{co}"), SB("gx")), (SB("u"),))
                    tt_(u_, u_, a2_, ALU.mult, (SB("u"), SB("a2")), (SB("u"),))
                    if first and t == 0:
                        init = 0.0
                        rd = ()
                    else:
                        init = chh[:, l, ch:ch + 1]
                        rd = (bchh[l][ch],)
                    P.op("dve", lambda e, init=init, a_=a_, u_=u_, hs_=hs_: e.tensor_tensor_scan(
                        out=hs_, data0=a_, data1=u_, initial=init, op0=ALU.mult, op1=ALU.add),
                        reads=(SB("a"), SB("u")) + rd, writes=(SB("hs"),))
                    act(chh[:, l, ch:ch + 1], hs_[:, TT - 1:TT], AF.Identity, (SB("hs"),), (bchh[l][ch],))
                    if not DBG.get('dump_r'):
                        tt_(ym[:, ch * T + t * TT: ch * T + (t + 1) * TT], hs_, gg[:, co, tsl], ALU.mult,
                            (SB("hs"), SB(f"gg{co}")), (bym[ch],))
        P.sync_all()
        for g in range(4):
            W = WINS[g]
            for ci in range(2):
                ch = 2 * g + ci
                if first:
                    P.op("dve", lambda e, ci=ci: e.memset(zx[:, ci, 0:HALO], 0.0), writes=(SB(f"zx{ci}"),))
                else:
                    act(zx[:, ci, 0:HALO], czp[:, l, ch, :], AF.Identity, (bczp[l][ch],), (SB(f"zx{ci}"),))
                wt, wb = wget("win", l * 24 + 16 + g * 2 + ci, 2048)
                for t in range(NTT):
                    bk, bbk = getbank()
                    for k in range(NCD):
                        mm(bk[:, :], wt[:, k * 128:(k + 1) * 128], hb[:, k, t * TT:(t + 1) * TT], k == 0, k == NCD - 1,
                           (wb, bh[k]), (bbk,), k == NCD - 1)
                    act(zx[:, ci, HALO + t * TT: HALO + (t + 1) * TT], bk[:, :], AF.Identity, (bbk,), (SB(f"zx{ci}"),))
                act(czp[:, l, ch, :], zx[:, ci, T:T + HALO], AF.Identity, (SB(f"zx{ci}"),), (bczp[l][ch],))
            for ci in range(2):
                src = zx[:, ci, :]
                srcb = SB(f"zx{ci}")
                pp = [(sA, SB("sA")), (sB, SB("sB"))]
                sh = 1
                nst = g + 1
                for st in range(nst):
                    dst, dstb = pp[st % 2]
                    tt_(dst[:, sh:T + HALO], src[:, sh:T + HALO], src[:, 0:T + HALO - sh], ALU.add,
                        (srcb,), (dstb,))
                    src, srcb = dst, dstb
                    sh *= 2
                stt(pb[:, ci, :], src[:, HALO:HALO + T], 1.0 / W, zx[:, ci, HALO:HALO + T], ALU.mult, ALU.subtract,
                    (srcb, SB(f"zx{ci}")), (SB(f"pb{ci}"),))
                if first:
                    tt_(tfix, src[:, HALO:2 * HALO], pvc("invcnt", g * 16, 16), ALU.mult, (srcb, bpv), (SB("tfix"),))
                    tt_(pb[:, ci, 0:HALO], tfix, zx[:, ci, HALO:2 * HALO], ALU.subtract,
                        (SB("tfix"), SB(f"zx{ci}")), (SB(f"pb{ci}"),))
            pw, pwb = wget("gat", l * 12 + 8 + g, 512)
            for co in range(2):
                ch = 2 * g + co
                for t in range(NTT):
                    tsl = slice(t * TT, (t + 1) * TT)
                    bk, bbk = getbank()
                    for ci in range(2):
                        mm(bk[:, :], pw[:, ci * 256 + co * 128: ci * 256 + co * 128 + 128], pb[:, ci, tsl],
                           ci == 0, ci == 1, (pwb, SB(f"pb{ci}")), (bbk,), ci == 1)
                    act(ym[:, (8 + ch) * T + t * TT: (8 + ch) * T + (t + 1) * TT], bk[:, :], AF.Identity,
                        (bbk, bpv, blrc), (bym[8 + ch],),
                        bias=lrc[:, l, 2, ch:ch + 1], scale=pvc(f"pools{l}", ch, 1))
        for m in range(NCD):
            wt, wb = wget("wout", l * 16 + m, 2048)
            for t in range(NTT):
                bk, bbk = getbank()
                for k in range(NCD):
                    mm(bk[:, :], wt[:, k * 128:(k + 1) * 128], ym[:, k * T + t * TT: k * T + (t + 1) * TT],
                       k == 0, k == NCD - 1, (wb, bym[k]), (bbk,), k == NCD - 1)
                residual_from_bank(bk, bbk, sidx, m, t)
        P.sync_all()
        if DBG.get('dump_ym') and l == 0 and first:
            P.dma('sp', dsm, ymd[:, :], ym[:, :], reads=bym)
        layer_norm(l, 0)
        P.sync_all()

    def h1v(bi, kk, t):
        return ym[:, (bi * GE + kk) * T + t * TT: (bi * GE + kk) * T + (t + 1) * TT]

    def ffn_group(sidx, kindgu, gu_base, kinddn, dn_base, G, bi, cw_e):
        for kk in range(G):
            wg, wgb = wget(kindgu, (gu_base + kk) * 2, 2048)
            wu, wub = wget(kindgu, (gu_base + kk) * 2 + 1, 2048)
            bks = []
            for (wt, wb) in ((wg, wgb), (wu, wub)):
                for t in range(NTT):
                    bk, bbk = getbank()
                    for k in range(NCD):
                        mm(bk[:, :], wt[:, k * 128:(k + 1) * 128], hb[:, k, t * TT:(t + 1) * TT], k == 0, k == NCD - 1,
                           (wb, bh[k]), (bbk,), k == NCD - 1)
                    bks.append((bk, bbk))
            for t in range(NTT):
                bg, bbg = bks[t]
                bu, bbu = bks[NTT + t]
                sg = ftm[:, t, :]
                act(sg, bg[:, :], AF.Silu, (bbg,), (SB(f"sg{t}"),))
                if cw_e is None:
                    tt_(h1v(bi, kk, t), bu[:, :], sg, ALU.mult, (bbu, SB(f"sg{t}")), (bh1[bi][kk],))
                else:
                    tt_(sg, sg, cwb[:, cw_e, t * TT:(t + 1) * TT], ALU.mult, (SB(f"sg{t}"), SB("cwb")), (SB(f"sg{t}"),))
                    tt_(h1v(bi, kk, t), bu[:, :], sg, ALU.mult, (bbu, SB(f"sg{t}")), (bh1[bi][kk],))
        for m in range(NCD):
            wt, wb = wget(kinddn, dn_base + m, G * 128)
            for t in range(NTT):
                bk, bbk = getbank()
                for kk in range(G):
                    mm(bk[:, :], wt[:, kk * 128:(kk + 1) * 128], h1v(bi, kk, t), kk == 0, kk == G - 1,
                       (wb, bh1[bi][kk]), (bbk,), kk == G - 1)
                residual_from_bank(bk, bbk, sidx, m, t)

    def ffn_dense(l):
        sidx = l * 2 + 1
        modulate(sidx)
        for grp in range(D_FF // 128 // GD):
            ffn_group(sidx, "fgu", grp * GD, "fdn", grp * 16, GD, grp % 2, None)
        P.sync_all()
        layer_norm(l, 1)
        P.sync_all()

    def ffn_moe(l):
        sidx = l * 2 + 1
        ident = pvc("ident")
        ones = pvc("ones")
        rw = pvc("router")
        for t in range(NTT):
            tsl = slice(t * TT, (t + 1) * TT)
            lb = [getbank() for _ in range(4)]
            for c in range(NCD):
                hf = ftm[:, 2 + (c % 2), :]
                act(hf, xb[:, c, tsl], AF.Identity, (bx[c][t], bder, bmod), (SB(f"hf{c % 2}"),),
                    bias=modp[:, sidx, c:c + 1], scale=der[:, sidx, c:c + 1])
                for blk in range(4):
                    mm(lb[blk][0][:, 0:8], hf[:, blk * 128:(blk + 1) * 128], rw[:, c * 8:(c + 1) * 8], c == 0, c == NCD - 1,
                       (SB(f"hf{c % 2}"), bpv), (lb[blk][1],), True)
            for blk in range(4):
                lg, mx, nv1, ex, sel, den, cw = (rtm[:, i, :] for i in range(1, 8))
                act(lg, lb[blk][0][:, 0:8], AF.Identity, (lb[blk][1],), (SB("lg"),))
                P.op("dve", lambda e, mx=mx, lg=lg: e.max(out=mx, in_=lg), reads=(SB("lg"),), writes=(SB("mx"),))
                ts(nv1[:, 0:1], mx[:, 0:1], -1.0, None, ALU.mult, None, (SB("mx"),), (SB("nv1"),))
                act(ex, lg, AF.Exp, (SB("lg"), SB("nv1")), (SB("ex"),), bias=nv1[:, 0:1])
                ts(sel, lg, mx[:, 1:2], None, ALU.is_ge, None, (SB("lg"), SB("mx")), (SB("sel"),))
                tt_(sel, sel, ex, ALU.mult, (SB("sel"), SB("ex")), (SB("sel"),))
                P.op("dve", lambda e, den=den, sel=sel: e.reduce_sum(out=den[:, 0:1], in_=sel, axis=mybir.AxisListType.X),
                     reads=(SB("sel"),), writes=(SB("den"),))
                P.op("dve", lambda e, den=den: e.reciprocal(out=den[:, 0:1], in_=den[:, 0:1]),
                     reads=(SB("den"),), writes=(SB("den"),))
                ts(cw, sel, den[:, 0:1], None, ALU.mult, None, (SB("sel"), SB("den")), (SB(f"cw{blk}"),))
                act(msc[:, blk * 8:(blk + 1) * 8], cw, AF.Identity, (SB(f"cw{blk}"),), (SB(f"cws{blk}"),))
            for e_ in range(NEXP):
                bk, bbk = getbank()
                for blk in range(4):
                    dg = msc[:, 32 + (blk % 2) * 128: 32 + (blk % 2) * 128 + 128]
                    ts(dg, ident, msc[:, blk * 8 + e_: blk * 8 + e_ + 1], None, ALU.mult, None,
                       (bpv, SB(f"cws{blk}")), (SB(f"dg{blk % 2}"),))
                    mm(bk[:, blk * 128:(blk + 1) * 128], ones, dg, True, True, (bpv, SB(f"dg{blk % 2}")), (bbk,), True)
                act(cwb[:, e_, tsl], bk[:, :], AF.Identity, (bbk,), (SB("cwb"),))
        modulate(sidx)
        gi = 0
        for e_ in range(NEXP):
            for grp in range(D_FFE // 128 // GE):
                ffn_group(sidx, "egu", e_ * 56 + grp * GE, "edn", (e_ * 7 + grp) * 16, GE, gi % 2, e_)
                gi += 1
        P.sync_all()
        layer_norm(l, 1)
        P.sync_all()

    xs = [es.enter_context(nc.semaphore("xs")), 0]
    osm = [es.enter_context(nc.semaphore("os")), 0]
    allx = [bx[c][t] for c in range(NCD) for t in range(NTT)]
    last_out = None
    for sq in range(n_seq):
        for tl in range(n_tiles_seq):
            ti = sq * n_tiles_seq + tl
            P.dma("sp", xs, xb[:, :, :], xin[ti].rearrange("p (c t) -> p c t", c=NCD, t=T), writes=allx)
            first = (tl == 0)
            if first:
                adaln(sq)
            for l in layers:
                mixer(l, first)
                if DBG.get('skip_ffn'):
                    pass
                elif l == 0:
                    ffn_dense(l)
                else:
                    ffn_moe(l)
            last_out = P.dma("sp", osm, yout[ti].rearrange("p (c t) -> p c t", c=NCD, t=T), xb[:, :, :], reads=allx)
    P.op("sp", lambda e: e.nop(), extra=(last_out,), signal=False)
    assert wst["next"] == len(plan), (wst["next"], len(plan))

    with nc.Block() as block:
        P.emit(block)
    es.close()
    return nc


def pack_weights(inp, layers):
    w = {}
    w["ada"] = np.concatenate([pack_cols(inp["ada_w"][l, s]) for l in (0, 1) for s in (0, 1)], axis=0)
    w["win"] = np.concatenate([pack_cols(inp["mix_w_in"][l], WIN_ORDER) for l in (0, 1)], axis=0)
    g = []
    for l in (0, 1):
        for hd in range(4):
            g.append(pack_sq(inp["lru_wa"][l, hd]))
            g.append(pack_sq(inp["lru_wx"][l, hd]))
        for gi in range(4):
            g.append(pack_sq(inp["pool_w"][l, gi]))
    w["gat"] = np.stack(g, axis=0)
    w["wout"] = np.concatenate([pack_cols(inp["mix_w_out"][l]) for l in (0, 1)], axis=0)
    fg = pack_cols(inp["ffn_w_gate"][0])
    fu = pack_cols(inp["ffn_w_up"][0])
    w["fgu"] = np.ascontiguousarray(np.stack([fg, fu], axis=1).reshape(-1, 128, 2048))
    w["fdn"] = pack_down(inp["ffn_w_down"][0], GD)
    if 1 in layers:
        eg = np.stack([pack_cols(inp["exp_w_gate"][0, e]) for e in range(NEXP)], axis=0)
        eu = np.stack([pack_cols(inp["exp_w_up"][0, e]) for e in range(NEXP)], axis=0)
        w["egu"] = np.ascontiguousarray(np.stack([eg, eu], axis=2).reshape(-1, 128, 2048))
        w["edn"] = np.concatenate([pack_down(inp["exp_w_down"][0, e], GE) for e in range(NEXP)], axis=0)
    else:
        w["egu"] = np.zeros((1, 128, 2048), np.float32)
        w["edn"] = np.zeros((1, 128, GE * 128), np.float32)
    return w


def run(inp, n_cores, seq_per_core, n_tiles_seq, layers, seq_list=None):
    inp = {k: np.asarray(v, np.float32) for k, v in inp.items()}
    w = pack_weights(inp, layers)
    wshapes = {k: v.shape for k, v in w.items()}
    if seq_list is None:
        seq_list = [[c * seq_per_core + i for i in range(seq_per_core)] for c in range(n_cores)]
    pvs = [build_pvec(inp, seq_list[c], (0, 1)) for c in range(n_cores)]
    npv = pvs[0].n
    nc = build_program(n_tiles_seq, seq_per_core, layers, wshapes, npv, pvs[0].off)
    in_maps = []
    for c in range(n_cores):
        xt = []
        for b in seq_list[c]:
            for tl in range(n_tiles_seq):
                xs_ = inp["x"][b, tl * T:(tl + 1) * T, :]
                xt.append(xs_.reshape(T, NCD, 128).transpose(2, 1, 0).reshape(128, NCD * T))
        m = {"xin": np.ascontiguousarray(np.stack(xt, axis=0)),
             "pvec": np.ascontiguousarray(np.concatenate(pvs[c].cols, axis=1))}
        m.update(w)
        in_maps.append(m)
    res = run_bass_kernel_spmd(nc, in_maps, core_ids=list(range(n_cores)))
    outs = {}
    if DBG.get("dump_ym"):
        DBG["ymd"] = np.asarray(res.results[0]["ymd"]).astype(np.float32)
    for c in range(n_cores):
        y = res.results[c]["yout"]
        i = 0
        for b in seq_list[c]:
            for tl in range(n_tiles_seq):
                yt = y[i].reshape(128, NCD, T).transpose(2, 1, 0).reshape(T, D)
                outs[(b, tl)] = yt
                i += 1
    return outs


def kernel(**inputs):
    Bn, S, _ = inputs["x"].shape
    spc = Bn // N_CORES
    outs = run(inputs, N_CORES, spc, S // T, (0, 1))
    out = np.zeros((Bn, S, D), np.float32)
    for (b, tl), v in outs.items():
        out[b, tl * T:(tl + 1) * T, :] = v
    return out
```
